# Optimizing a Trainium2 kernel written in Bass

```python
import jax, jax.numpy as jnp
from jax import lax
import numpy as np

D_MODEL = 2048
BATCH = 2
SEQ = 8192
DEPTH = 2

M_HEADS = 4
M_HEAD_DIM = 256
W_M = M_HEADS * M_HEAD_DIM
SHORT_K = 3
CHUNK = 128
W_C = 1024
CONV_K = 31
D_FF = 5632
N_EXPERTS = 8
TOP_K = 2
D_FF_E = 7168
MOE_BLOCK = 256
N_DENSE = (DEPTH + 1) // 2
N_MOE = DEPTH // 2
EPS = 1e-6

OFF_Q = 0
OFF_K = W_M
OFF_V = 2 * W_M
OFF_O = 3 * W_M
OFF_G = 4 * W_M
OFF_GLU = OFF_G + 4 * M_HEADS
OFF_BR = OFF_GLU + 2 * W_C
N_IN = OFF_BR + 2 * D_MODEL

kernel_name = "hybrid_mlstm_conformer_moe_encoder"


def rms_norm(x):
    xf = x.astype(jnp.float32)
    return (xf * lax.rsqrt(jnp.mean(xf * xf, axis=-1, keepdims=True) + EPS)).astype(x.dtype)


def layer_norm(x, g, b):
    xf = x.astype(jnp.float32)
    mu = jnp.mean(xf, axis=-1, keepdims=True)
    var = jnp.mean(jnp.square(xf - mu), axis=-1, keepdims=True)
    y = (xf - mu) * lax.rsqrt(var + EPS)
    return (y * g.astype(jnp.float32) + b.astype(jnp.float32)).astype(x.dtype)


def depthwise_conv(x, w):
    pad = w.shape[0] // 2
    return lax.conv_general_dilated(x, w[:, None, :].astype(x.dtype), window_strides=(1,),
                                    padding=[(pad, pad)],
                                    dimension_numbers=('NWC', 'WIO', 'NWC'),
                                    feature_group_count=x.shape[-1])


def mlstm_scan(q, k, v, i_pre, f_pre):
    B, S, H, Dh = q.shape
    NC = S // CHUNK
    k = k * (Dh ** -0.5)
    logf = jax.nn.log_sigmoid(f_pre)

    def chunks(a):
        return a.reshape(B, NC, CHUNK, H, Dh).transpose(1, 0, 3, 2, 4)

    def gchunks(a):
        return a.reshape(B, NC, CHUNK, H).transpose(1, 0, 3, 2)

    qc, kc, vc = chunks(q), chunks(k), chunks(v)
    ic = gchunks(i_pre)
    bc = jnp.cumsum(gchunks(logf), axis=-1)
    tril = jnp.tril(jnp.ones((CHUNK, CHUNK), dtype=bool))

    def step(carry, xs):
        C, n, m = carry
        qb, kb, vb, ib, bb = xs
        dmat = bb[..., :, None] - bb[..., None, :] + ib[..., None, :]
        dmat = jnp.where(tril, dmat, -jnp.inf)
        inter = bb + m[..., None]
        m_t = jnp.maximum(inter, jnp.max(dmat, axis=-1))
        w_intra = jnp.exp(dmat - m_t[..., None])
        w_inter = jnp.exp(inter - m_t)
        s = jnp.einsum('bhtd,bhsd->bhts', qb, kb) * w_intra
        num = jnp.einsum('bhts,bhse->bhte', s, vb) + \
            w_inter[..., None] * jnp.einsum('bhtd,bhde->bhte', qb, C)
        den = jnp.sum(s, axis=-1) + w_inter * jnp.einsum('bhtd,bhd->bht', qb, n)
        h = num / jnp.maximum(jnp.abs(den), jnp.exp(-m_t))[..., None]
        m_new = m_t[..., -1]
        w_s = jnp.exp(bb[..., -1:] - bb + ib - m_new[..., None])
        carry_decay = jnp.exp(bb[..., -1] + m - m_new)
        kw = kb * w_s[..., None]
        C_new = carry_decay[..., None, None] * C + jnp.einsum('bhsd,bhse->bhde', kw, vb)
        n_new = carry_decay[..., None] * n + jnp.sum(kw, axis=2)
        return (C_new, n_new, m_new), h

    init = (jnp.zeros((B, H, Dh, Dh), jnp.float32), jnp.zeros((B, H, Dh), jnp.float32),
            jnp.zeros((B, H), jnp.float32))
    _, hs = lax.scan(step, init, (qc, kc, vc, ic, bc))
    return hs.transpose(1, 0, 3, 2, 4).reshape(B, S, H, Dh)


def mixer(h, w_in, b_in, w_qk_conv, m_norm_g, w_m_proj, w_dw, b_dw, ln_c_g, ln_c_b, w_c_proj, w_out):
    B, S, _ = h.shape
    z = h @ w_in + b_in
    qk = depthwise_conv(z[..., OFF_Q:OFF_V], w_qk_conv)
    q = qk[..., :W_M].reshape(B, S, M_HEADS, M_HEAD_DIM).astype(jnp.float32)
    k = qk[..., W_M:].reshape(B, S, M_HEADS, M_HEAD_DIM).astype(jnp.float32)
    v = z[..., OFF_V:OFF_O].reshape(B, S, M_HEADS, M_HEAD_DIM).astype(jnp.float32)
    o = z[..., OFF_O:OFF_G]
    gif = z[..., OFF_G:OFF_GLU].astype(jnp.float32).reshape(B, S, 2, 2, M_HEADS)
    h_fwd = mlstm_scan(q, k, v, gif[:, :, 0, 0], gif[:, :, 0, 1])
    rev = lambda a: jnp.flip(a, axis=1)
    h_bwd = rev(mlstm_scan(rev(q), rev(k), rev(v), rev(gif[:, :, 1, 0]), rev(gif[:, :, 1, 1])))
    hs = h_fwd + h_bwd
    hs = hs * lax.rsqrt(jnp.mean(hs * hs, axis=-1, keepdims=True) + EPS) * \
        m_norm_g.astype(jnp.float32).reshape(M_HEADS, M_HEAD_DIM)
    hm = hs.reshape(B, S, W_M).astype(h.dtype) * jax.nn.sigmoid(o)
    y_m = hm @ w_m_proj
    glu = z[..., OFF_GLU:OFF_BR]
    u = glu[..., :W_C] * jax.nn.sigmoid(glu[..., W_C:])
    u = depthwise_conv(u, w_dw) + b_dw
    u = jax.nn.silu(layer_norm(u, ln_c_g, ln_c_b))
    y_c = u @ w_c_proj
    gates = jax.nn.sigmoid(z[..., OFF_BR:])
    merged = gates[..., :D_MODEL] * y_m + gates[..., D_MODEL:] * y_c
    return merged @ w_out


def swiglu(h, w13, w2):
    a = h @ w13
    f = a.shape[-1] // 2
    return (jax.nn.silu(a[..., :f]) * a[..., f:]) @ w2


def moe_ffn(h, router, w13, w2):
    B, S, D = h.shape
    T = B * S
    A = T * TOP_K
    xt = h.reshape(T, D)
    logits = (xt @ router).astype(jnp.float32)
    top_v, top_e = lax.top_k(logits, TOP_K)
    gate_w = jax.nn.softmax(top_v, axis=-1)
    flat_e = top_e.reshape(-1)
    flat_tok = jnp.repeat(jnp.arange(T, dtype=jnp.int32), TOP_K)
    flat_w = gate_w.reshape(-1)
    order = jnp.argsort(flat_e, stable=True)
    se = flat_e[order]
    counts = jnp.zeros((N_EXPERTS,), jnp.int32).at[flat_e].add(1)
    padded = ((counts + MOE_BLOCK - 1) // MOE_BLOCK) * MOE_BLOCK
    pad_end = jnp.cumsum(padded)
    pad_start = pad_end - padded
    start = jnp.cumsum(counts) - counts
    dest = pad_start[se] + (jnp.arange(A, dtype=jnp.int32) - start[se])
    NB = (A + MOE_BLOCK - 1) // MOE_BLOCK + N_EXPERTS
    P = NB * MOE_BLOCK
    buf_tok = jnp.zeros((P,), jnp.int32).at[dest].set(flat_tok[order])
    buf_w = jnp.zeros((P,), jnp.float32).at[dest].set(flat_w[order])
    blk_e = jnp.minimum(jnp.searchsorted(pad_end, jnp.arange(NB, dtype=jnp.int32) * MOE_BLOCK,
                                         side='right'), N_EXPERTS - 1)
    xb = xt[buf_tok].reshape(NB, MOE_BLOCK, D)

    def expert_block(args):
        xblk, e = args
        return swiglu(xblk, w13[e], w2[e])

    yb = lax.map(expert_block, (xb, blk_e)).reshape(P, D)
    out = jnp.zeros((T, D), h.dtype).at[buf_tok].add(yb * buf_w[:, None].astype(yb.dtype))
    return out.reshape(B, S, D)


def setup_inputs(seed: int = 0) -> dict:
    key = jax.random.key(seed)
    ks = jax.random.split(key, 24)
    nrm = lambda k, shape, s: jax.random.normal(k, shape, jnp.float32) * s
    D = D_MODEL
    gk = jax.random.split(ks[4], 4)
    i_bias = nrm(gk[0], (DEPTH, 2, 1, M_HEADS), 0.1)
    f_bias = jnp.linspace(3.0, 6.0, M_HEADS, dtype=jnp.float32)[None, None, None, :] + \
        nrm(gk[1], (DEPTH, 2, 1, M_HEADS), 0.1)
    gif_bias = jnp.concatenate([i_bias, f_bias], axis=2).reshape(DEPTH, 4 * M_HEADS)
    b_in = jnp.concatenate([nrm(gk[2], (DEPTH, OFF_G), 0.02), gif_bias,
                            nrm(gk[3], (DEPTH, N_IN - OFF_GLU), 0.02)], axis=-1)
    return {
        "x": nrm(ks[0], (BATCH, SEQ, D), 1.0),
        "c": nrm(ks[1], (BATCH, D), 1.0),
        "w_mod": nrm(ks[2], (DEPTH, D, 6 * D), 0.5 * D ** -0.5),
        "b_mod": nrm(ks[3], (DEPTH, 6 * D), 0.02),
        "w_in": nrm(ks[5], (DEPTH, D, N_IN), D ** -0.5),
        "b_in": b_in,
        "w_qk_conv": nrm(ks[6], (DEPTH, SHORT_K, 2 * W_M), SHORT_K ** -0.5),
        "m_norm_g": 1.0 + nrm(ks[7], (DEPTH, W_M), 0.02),
        "w_m_proj": nrm(ks[8], (DEPTH, W_M, D), W_M ** -0.5),
        "w_dw": nrm(ks[9], (DEPTH, CONV_K, W_C), CONV_K ** -0.5),
        "b_dw": nrm(ks[10], (DEPTH, W_C), 0.02),
        "ln_c_g": 1.0 + nrm(ks[11], (DEPTH, W_C), 0.02),
        "ln_c_b": nrm(ks[12], (DEPTH, W_C), 0.02),
        "w_c_proj": nrm(ks[13], (DEPTH, W_C, D), W_C ** -0.5),
        "w_out": nrm(ks[14], (DEPTH, D, D), D ** -0.5),
        "ffn_w13": nrm(ks[15], (N_DENSE, D, 2 * D_FF), D ** -0.5),
        "ffn_w2": nrm(ks[16], (N_DENSE, D_FF, D), D_FF ** -0.5),
        "moe_router": nrm(ks[17], (N_MOE, D, N_EXPERTS), D ** -0.5),
        "moe_w13": nrm(ks[18], (N_MOE, N_EXPERTS, D, 2 * D_FF_E), D ** -0.5),
        "moe_w2": nrm(ks[19], (N_MOE, N_EXPERTS, D_FF_E, D), D_FF_E ** -0.5),
        "final_g": 1.0 + nrm(ks[20], (D,), 0.02),
    }


def reference(x, c, w_mod, b_mod, w_in, b_in, w_qk_conv, m_norm_g, w_m_proj, w_dw, b_dw,
              ln_c_g, ln_c_b, w_c_proj, w_out, ffn_w13, ffn_w2, moe_router, moe_w13, moe_w2,
              final_g):
    c_act = jax.nn.silu(c)
    for l in range(DEPTH):
        mod = c_act @ w_mod[l] + b_mod[l]
        sh1, sc1, g1, sh2, sc2, g2 = [m[:, None, :] for m in jnp.split(mod, 6, axis=-1)]
        h = rms_norm(x) * (1.0 + sc1) + sh1
        x = x + g1 * mixer(h, w_in[l], b_in[l], w_qk_conv[l], m_norm_g[l], w_m_proj[l],
                           w_dw[l], b_dw[l], ln_c_g[l], ln_c_b[l], w_c_proj[l], w_out[l])
        h = rms_norm(x) * (1.0 + sc2) + sh2
        if l % 2 == 0:
            f = swiglu(h, ffn_w13[l // 2], ffn_w2[l // 2])
        else:
            f = moe_ffn(h, moe_router[l // 2], moe_w13[l // 2], moe_w2[l // 2])
        x = x + g2 * f
    return rms_norm(x) * final_g
```

```python
import contextlib
import numpy as np
import concourse.bass as bass
import concourse.mybir as mybir
from concourse.bass_utils import run_bass_kernel_spmd

F32 = mybir.dt.float32
BF16 = mybir.dt.bfloat16
AF = mybir.ActivationFunctionType
ALU = mybir.AluOpType
AX = mybir.AxisListType
ENGS = ("pe", "act", "dve", "pool", "sp")
NSLOT = 8
GROUPS = [[0, 1, 2, 3], [4, 5, 6, 7]]
EPS = 1e-6
NEG = -30000.0

FULL = dict(D=2048, T=2048, DFF=5632, DFFE=7168, L=2)


class KB:
    def __init__(self, nc):
        self.nc = nc
        self.ops = {e: [] for e in ENGS}
        self.last_w = {}
        self.readers = {}
        self.ndma = {e: 0 for e in ENGS}
        self.ncc = 0
        self.pending = {}

    def barrier(self):
        evs = set()
        for e in ENGS:
            ops = self.ops[e]
            seen = {"c": 0, "d": 0, "cc": 0}
            lim = {"c": 1, "d": NSLOT, "cc": 1}
            for i in range(len(ops) - 1, -1, -1):
                kd = ops[i]["kind"]
                if seen[kd] < lim[kd]:
                    evs.add((e, i))
                    seen[kd] += 1
                if all(seen[x] >= lim[x] for x in seen):
                    break
        self.pending = {e: set(evs) for e in ENGS}

    def op(self, eng, fn, reads=(), writes=(), kind="c"):
        deps = set(self.pending.pop(eng, ()))
        for r in reads:
            if r in self.last_w:
                deps.add(self.last_w[r])
        for w in writes:
            if w in self.last_w:
                deps.add(self.last_w[w])
            deps |= self.readers.get(w, set())
        idx = len(self.ops[eng])
        ev = (eng, idx)
        dn = None
        if kind == "d":
            dn = self.ndma[eng]
            self.ndma[eng] += 1
        if kind == "cc":
            dn = self.ncc
            self.ncc += 1
        self.ops[eng].append(dict(fn=fn, deps=deps, kind=kind, dn=dn))
        for r in reads:
            self.readers.setdefault(r, set()).add(ev)
        for w in writes:
            self.last_w[w] = ev
            self.readers[w] = set()
        return ev

    def emit(self):
        nc = self.nc
        with contextlib.ExitStack() as st:
            csem = {e: st.enter_context(nc.semaphore("c_" + e)) for e in ENGS}
            dsem = {e: [st.enter_context(nc.semaphore(f"d_{e}{i}")) for i in range(NSLOT)]
                    for e in ("sp", "pool")}
            ccsem = st.enter_context(nc.semaphore("ccsem"))
            block = st.enter_context(nc.Block())
            ccount = {}
            for e in ENGS:
                c = 0
                for i, o in enumerate(self.ops[e]):
                    if o["kind"] == "c":
                        c += 1
                        ccount[(e, i)] = c

            def target(ev):
                e, i = ev
                o = self.ops[e][i]
                if o["kind"] == "c":
                    return csem[e], ccount[ev]
                dn = o["dn"]
                if o["kind"] == "cc":
                    return ccsem, dn + 1
                return dsem[e][dn % NSLOT], 16 * (dn // NSLOT + 1)

            def run(eng_name, eng):
                waited = {}
                for i, o in enumerate(self.ops[eng_name]):
                    tg = [target(d) for d in o["deps"]]
                    if o["kind"] == "d" and o["dn"] >= NSLOT:
                        dn = o["dn"]
                        tg.append((dsem[eng_name][dn % NSLOT], 16 * (dn // NSLOT)))
                    for s, v in tg:
                        key = id(s)
                        if waited.get(key, 0) >= v:
                            continue
                        waited[key] = v
                        eng.wait_ge(s, v)
                    ins = o["fn"](eng)
                    if o["kind"] == "c":
                        ins.then_inc(csem[eng_name], 1)
                    elif o["kind"] == "cc":
                        ins.then_inc(ccsem, 1)
                    else:
                        ins.then_inc(dsem[eng_name][o["dn"] % NSLOT], 16)
                if eng_name == "pool" and self.ncc > 0:
                    eng.wait_ge(ccsem, self.ncc)
                if eng_name in dsem:
                    n = self.ndma[eng_name]
                    for sl in range(NSLOT):
                        cnt = (n - sl + NSLOT - 1) // NSLOT if n > sl else 0
                        if cnt > 0:
                            eng.wait_ge(dsem[eng_name][sl], 16 * cnt)

            @block.tensor
            def _(e):
                run("pe", e)

            @block.scalar
            def _(e):
                run("act", e)

            @block.vector
            def _(e):
                run("dve", e)

            @block.gpsimd
            def _(e):
                run("pool", e)

            @block.sync
            def _(e):
                run("sp", e)


class Arena:
    def __init__(self, ap, width):
        self.ap, self.W, self.off, self.gen = ap, width, 0, 0

    def reset(self):
        self.off = 0
        self.gen += 1

    def alloc(self, name, shape, dt=F32):
        n = 1
        for d in shape[1:]:
            n *= d
        ne = n + (n % 2)
        nf = ne if dt == F32 else ne // 2
        nfa = (nf + 7) // 8 * 8
        o = self.off
        self.off += nfa
        assert self.off <= self.W, f"arena overflow at {name}: {self.off} > {self.W}"
        v = self.ap[0:shape[0], o:o + nf]
        if dt == BF16:
            v = v.bitcast(BF16)
        if ne != n:
            v = v[:, 0:n]
        if len(shape) == 3:
            v = v.rearrange("p (a b) -> p a b", b=shape[2])
        elif len(shape) == 4:
            v = v.rearrange("p (a b c) -> p a b c", b=shape[2], c=shape[3])
        return v, f"{name}#{self.gen}"


def build(cfg, stop_after="all"):
    D, T, DFF, DFFE, L = cfg["D"], cfg["T"], cfg["DFF"], cfg["DFFE"], cfg["L"]
    DC = D // 128
    S = 4 * T
    NCH = S // 128
    NMC = 6 * DC // 4
    FC = DFF // 128
    FCE = DFFE // 128
    TTN = 256
    TW = min(512, T)
    NTP = 1540
    AW = 45056
    nc = bass.Bass("TRN2", target_bir_lowering=False)
    k = KB(nc)

    def din(name, shape, dt=F32):
        return nc.dram_tensor(name, list(shape), dt, kind="ExternalInput").ap()

    def dscr(name, shape, dt=F32):
        return nc.dram_tensor(name, list(shape), dt).ap()

    xT = din("xT", [D, T])
    c_col = din("c_col", [128, DC])
    consts = din("consts", [128, 6 * 128])
    w_mod_q = din("w_mod_q", [L, D, NMC * 128])
    b_mod_q = din("b_mod_q", [128, L * NMC])
    w_in_tp = din("w_in_tp", [L, D, NTP])
    b_tp_col = din("b_tp_col", [128, L * 8])
    b_tp_row = din("b_tp_row", [L, 516])
    wqk = din("wqk", [128, L * 12])
    mng = din("mng", [L, 256])
    w_mp = din("w_mp", [L, 256, D])
    w_dw = din("w_dw", [128, L * 62])
    cvec = din("cvec", [128, L * 6])
    w_cp = din("w_cp", [L, 256, D])
    w_br_q = din("w_br_q", [L, 2 * DC * 32, DC * 128])
    b_br_col = din("b_br_col", [128, L * 2 * DC])
    w_out_q = din("w_out_q", [L, DC * 32, DC * 128])
    ffn1_q = din("ffn1_q", [FC * 32, DC * 128])
    ffn3_q = din("ffn3_q", [FC * 32, DC * 128])
    NG_ = next(n for n in range((FC + 15) // 16, FC + 1) if FC % n == 0)
    ffn2_q = din("ffn2_q", [DC * NG_ * 32, (FC // NG_) * 128])
    router = din("router", [128, DC * 8])
    sele = din("sele", [8, 256])
    moe13 = din("moe13", [2, FCE * 128, DC * 256])
    moe2 = din("moe2", [2, DC * 128, FCE * 128])
    fg_col = din("fg_col", [128, DC])
    yT = nc.dram_tensor("yT", [D, T], F32, kind="ExternalOutput").ap()

    xres = dscr("xres", [D, T])
    mod_in = dscr("mod_in", [128, L * NMC])
    mod_all = dscr("mod_all", [4 * 128, L * NMC])
    hown = dscr("hown", [D, T], BF16)
    RCH = min(D, max(128, (1 << 20) // (T * 2) // 128 * 128))
    NPH = D // RCH
    hfull = dscr("hfull", [NPH, 4, RCH, T], BF16)
    zqk = dscr("zqk", [512, S])
    qkd = dscr("qkd", [512, S], BF16)
    ud = dscr("ud", [256, S + 30])
    vd = dscr("vd", [S, 258], BF16)
    sod = dscr("sod", [S, 256], BF16)
    hfd = dscr("hfd", [S, 256])
    cvd = dscr("cvd", [256, S])
    hmd = dscr("hmd", [256, S], BF16)
    ucd = dscr("ucd", [256, S], BF16)
    st1_in = dscr("st1_in", [1, S])
    st2_in = dscr("st2_in", [1, S])
    st1_all = dscr("st1_all", [4, S])
    st2_all = dscr("st2_all", [4, S])
    rs_in = dscr("rs_in", [4 * 2 * D, T])
    rs_out = dscr("rs_out", [2 * D, T])
    rs2_in = dscr("rs2_in", [4 * D, T])
    rs2_out = dscr("rs2_out", [D, T])
    gate_in = dscr("gate_in", [8, T])
    gate_all = dscr("gate_all", [32, T])
    NG = next(n for n in range((FC + 15) // 16, FC + 1) if FC % n == 0)
    GF = FC // NG
    R_BR, R_WO, R_F1, R_F2 = 2 * DC * 32, DC * 32, FC * 32, DC * NG * 32
    for R_ in (R_BR, R_WO, R_F1, R_F2):
        assert R_ % 128 == 0
    wbr_in = [dscr(f"wbr_in{l}", [R_BR, DC * 128]) for l in range(L)]
    wbr = [dscr(f"wbr{l}", [R_BR // 128, 4, 128, DC * 128]) for l in range(L)]
    wo_in = [dscr(f"wo_in{l}", [R_WO, DC * 128]) for l in range(L)]
    wo = [dscr(f"wo{l}", [R_WO // 128, 4, 128, DC * 128]) for l in range(L)]
    f1_in = dscr("f1_in", [R_F1, DC * 128])
    f1 = dscr("f1", [R_F1 // 128, 4, 128, DC * 128])
    f3_in = dscr("f3_in", [R_F1, DC * 128])
    f3 = dscr("f3", [R_F1 // 128, 4, 128, DC * 128])
    f2_in = dscr("f2_in", [R_F2, GF * 128])
    f2 = dscr("f2", [R_F2 // 128, 4, 128, GF * 128])

    def gtile(G, R_, blk):
        r_ = (blk * 128) // R_
        n_ = ((blk * 128) % R_) // 128
        return G[n_, r_]

    def fm(ap):
        return ap.rearrange("(c p) t -> p c t", p=128)

    with contextlib.ExitStack() as st:
        def sb(name, shape, dt=F32):
            return st.enter_context(nc.sbuf_tensor(name, list(shape), dt))

        def dma(q, out, in_, reads=(), writes=()):
            k.op(q, lambda e: e.dma_start(out=out, in_=in_), reads=reads, writes=writes, kind="d")

        def cc(kind, inp, outp, reads, writes):
            alu = ALU.bypass if kind == "AllGather" else ALU.add
            k.op("pool", lambda e: e.collective_compute(kind, alu, replica_groups=GROUPS,
                                                        ins=[inp], outs=[outp]),
                 reads=reads, writes=writes, kind="cc")

        def ag_chunked(src, srcn, out4, outn, rc):
            for n_ in range(out4.shape[0]):
                cc("AllGather", src[n_ * rc:(n_ + 1) * rc, :], out4[n_].rearrange("r i c -> (r i) c"), [srcn], [outn])

        def cast_load(dst3, src2, inner, reads, writes):
            A_ = dst3.shape[1]
            step = max(1, 1024 // inner)
            for a0 in range(0, A_, step):
                a1 = min(A_, a0 + step)
                dma("pool", dst3[:, a0:a1, :], src2[:, a0 * inner:a1 * inner].rearrange("p (c n) -> p c n", n=inner),
                    reads=reads, writes=writes)

        def hload(tile3, tn, seg, tsl):
            qn = RCH // 128
            for n_ in range(NPH):
                dma("sp", tile3[:, n_ * qn:(n_ + 1) * qn, :], hfull[n_, seg].rearrange("(q p) t -> p q t", p=128)[:, :, tsl],
                    reads=["hfull"], writes=[tn])

        ps = [st.enter_context(nc.psum_tensor(f"ps{i}", [128, 512], F32)) for i in range(6)]
        pst = st.enter_context(nc.psum_tensor("pst", [128, 512], BF16))
        psn = [0]

        def nps():
            psn[0] = (psn[0] + 1) % 6
            return ps[psn[0]], f"ps{psn[0]}"

        def mm(out, pairs, reads, writes):
            def fn(e):
                ins = None
                n = len(pairs)
                for j, (a, b) in enumerate(pairs):
                    ins = e.matmul(out, a, b, start=(j == 0), stop=(j == n - 1))
                return ins
            k.op("pe", fn, reads=reads, writes=writes)

        def dve(fn, reads, writes):
            k.op("dve", fn, reads=reads, writes=writes)

        def act(fn, reads, writes):
            k.op("act", fn, reads=reads, writes=writes)

        cst = sb("cst", [128, 6 * 128])
        dma("sp", cst[:], consts, writes=["cst"])
        Umat, Lmat = cst[:, 0:128], cst[:, 128:256]
        negf, negb = cst[:, 256:384], cst[:, 384:512]
        identf, onesf = cst[:, 512:640], cst[:, 640:768]
        identb = sb("identb", [128, 128], BF16)
        dve(lambda e: e.tensor_copy(out=identb[:], in_=identf), ["cst"], ["identb"])
        cact = sb("cact", [128, DC])
        bmod = sb("bmod", [128, L * NMC])
        modp = sb("modp", [128, L * NMC])
        modall = sb("modall", [128, 4, L * NMC])
        modp1 = sb("modp1", [128, 4, L * NMC])
        g4 = sb("g4", [128, NCH, 4])
        lf = sb("lf", [128, NCH, 2])
        tmpg = sb("tmpg", [128, NCH, 2])
        btc = sb("btc", [128, 8])
        btr = sb("btr", [128, 516])
        wq = sb("wq", [128, 12])
        mngb = sb("mngb", [128, 256])
        wdw = sb("wdw", [128, 62])
        cv = sb("cv", [128, 6])
        bbr = sb("bbr", [128, L * 2 * DC])
        fgc = sb("fgc", [128, DC])
        rtr = sb("rtr", [128, DC, 8])
        selt = sb("selt", [8, 256])
        arena_t = sb("arena", [128, AW])
        A = Arena(arena_t[:, :], AW)
        dma("sp", bbr[:], b_br_col, writes=["bbr"])
        dma("sp", fgc[:], fg_col, writes=["fgc"])
        dma("sp", rtr[:], router.rearrange("p (c e) -> p c e", e=8), writes=["rtr"])
        dma("sp", selt[:], sele, writes=["selt"])

        def phase():
            k.barrier()
            A.reset()

        for l in range(L):
            dma("pool", wbr_in[l], w_br_q[l], writes=[f"wbr_in{l}"])
            ag_chunked(wbr_in[l], f"wbr_in{l}", wbr[l], f"wbr{l}", 128)
            dma("pool", wo_in[l], w_out_q[l], writes=[f"wo_in{l}"])
            ag_chunked(wo_in[l], f"wo_in{l}", wo[l], f"wo{l}", 128)
        dma("pool", f1_in, ffn1_q, writes=["f1_in"])
        ag_chunked(f1_in, "f1_in", f1, "f1", 128)
        dma("pool", f3_in, ffn3_q, writes=["f3_in"])
        ag_chunked(f3_in, "f3_in", f3, "f3", 128)
        dma("pool", f2_in, ffn2_q, writes=["f2_in"])
        ag_chunked(f2_in, "f2_in", f2, "f2", 128)

        dma("sp", xres, xT, writes=["xres"])

        dma("sp", cact[:], c_col, writes=["cact"])
        act(lambda e: e.activation(out=cact[:], in_=cact[:], func=AF.Silu), ["cact"], ["cact"])
        dma("sp", bmod[:], b_mod_q, writes=["bmod"])
        wmt, wmtn = A.alloc("wmt", [128, DC, 128])
        for l in range(L):
            for m in range(NMC):
                src = w_mod_q[l].rearrange("(c p) n -> p c n", p=128)[:, :, m * 128:(m + 1) * 128]
                dma("sp", wmt, src, writes=[wmtn])
                p_, pn = nps()
                mm(p_[:, 0:1], [(wmt[:, kc, :], cact[:, kc:kc + 1]) for kc in range(DC)], [wmtn, "cact"], [pn])
                col = l * NMC + m
                dve(lambda e, p_=p_, col=col: e.tensor_tensor(
                    out=modp[:, col:col + 1], in0=p_[:, 0:1], in1=bmod[:, col:col + 1], op=ALU.add),
                    [pn, "bmod"], ["modp"])
        dma("sp", mod_in, modp[:], reads=["modp"], writes=["mod_in"])
        cc("AllGather", mod_in, mod_all, ["mod_in"], ["mod_all"])
        dma("sp", modall[:], mod_all.rearrange("(r p) c -> p r c", p=128), reads=["mod_all"], writes=["modall"])
        dve(lambda e: e.tensor_scalar(out=modp1[:], in0=modall[:], scalar1=1.0, scalar2=None, op0=ALU.add),
            ["modall"], ["modp1"])

        def mcol(t, l, j, dc):
            g = j * DC + dc
            return t[:, g // NMC, l * NMC + (g % NMC):l * NMC + (g % NMC) + 1]

        def norm_mod(l, jsh, jsc, route=False):
            phase()
            xt, xtn = A.alloc("xt", [128, DC, TTN])
            sqt, sqtn = A.alloc("sqt", [128, DC, TTN])
            rstd, rstdn = A.alloc("rstd", [128, TTN])
            tmpf, tmpfn = A.alloc("tmpf", [128, TTN])
            hx, hxn = A.alloc("hx", [128, DC, TTN], BF16)
            if route:
                hf, hfn = A.alloc("hf", [128, DC, TTN])
                lg, lgn = A.alloc("lg", [128, 8])
                eq, eqn = A.alloc("eq", [128, 8])
                l2, l2n = A.alloc("l2", [128, 8])
                ex, exn = A.alloc("ex", [128, 8])
                m1, m1n = A.alloc("m1", [128, 2])
                m2, m2n = A.alloc("m2", [128, 2])
                den, denn = A.alloc("den", [128, 2])
                gT, gTn = A.alloc("gT", [8, T])
            for tt in range(T // TTN):
                tsl = slice(tt * TTN, (tt + 1) * TTN)
                dma("sp", xt, fm(xres)[:, :, tsl], reads=["xres"], writes=[xtn])
                act(lambda e: e.activation(out=sqt, in_=xt, func=AF.Square), [xtn], [sqtn])
                p_, pn = nps()
                mm(p_[:, 0:TTN], [(onesf, sqt[:, dc, :]) for dc in range(DC)], [sqtn, "cst"], [pn])
                act(lambda e, p_=p_: e.activation(out=rstd, in_=p_[:, 0:TTN], func=AF.Sqrt, scale=1.0 / D, bias=EPS),
                    [pn], [rstdn])
                dve(lambda e: e.reciprocal(out=rstd, in_=rstd), [rstdn], [rstdn])
                for dc in range(DC):
                    dve(lambda e, dc=dc: e.tensor_tensor(out=tmpf, in0=xt[:, dc, :], in1=rstd, op=ALU.mult),
                        [xtn, rstdn], [tmpfn])
                    if route:
                        dve(lambda e, dc=dc: e.tensor_scalar(
                            out=hf[:, dc, :], in0=tmpf, scalar1=mcol(modp1, l, jsc, dc), scalar2=mcol(modall, l, jsh, dc),
                            op0=ALU.mult, op1=ALU.add), [tmpfn, "modp1", "modall"], [hfn])
                        dve(lambda e, dc=dc: e.tensor_copy(out=hx[:, dc, :], in_=hf[:, dc, :]), [hfn], [hxn])
                    else:
                        dve(lambda e, dc=dc: e.tensor_scalar(
                            out=hx[:, dc, :], in0=tmpf, scalar1=mcol(modp1, l, jsc, dc), scalar2=mcol(modall, l, jsh, dc),
                            op0=ALU.mult, op1=ALU.add), [tmpfn, "modp1", "modall"], [hxn])
                dma("sp", fm(hown)[:, :, tsl], hx, reads=[hxn], writes=["hown"])
                if route:
                    for tb in range(TTN // 128):
                        c0 = tt * TTN + tb * 128
                        pl, pln = nps()
                        mm(pl[:, 0:8], [(hf[:, dc, tb * 128:(tb + 1) * 128], rtr[:, dc, :]) for dc in range(DC)],
                           [hfn, "rtr"], [pln])
                        dve(lambda e, pl=pl: e.tensor_copy(out=lg, in_=pl[:, 0:8]), [pln], [lgn])
                        dve(lambda e: e.tensor_reduce(out=m1[:, 0:1], in_=lg, axis=AX.X, op=ALU.max), [lgn], [m1n])
                        dve(lambda e: e.tensor_scalar(out=eq, in0=lg, scalar1=m1[:, 0:1], scalar2=None, op0=ALU.is_ge), [lgn, m1n], [eqn])
                        dve(lambda e: e.scalar_tensor_tensor(out=l2, in0=eq, scalar=-1e30, in1=lg, op0=ALU.mult, op1=ALU.add),
                            [eqn, lgn], [l2n])
                        dve(lambda e: e.tensor_reduce(out=m2[:, 0:1], in_=l2, axis=AX.X, op=ALU.max), [l2n], [m2n])
                        dve(lambda e: e.tensor_scalar(out=eq, in0=lg, scalar1=m2[:, 0:1], scalar2=None, op0=ALU.is_ge), [lgn, m2n], [eqn])
                        dve(lambda e: e.tensor_scalar(out=m1[:, 1:2], in0=m1[:, 0:1], scalar1=-1.0, scalar2=None, op0=ALU.mult), [m1n], [m1n])
                        act(lambda e: e.activation(out=ex, in_=lg, func=AF.Exp, bias=m1[:, 1:2]), [lgn, m1n], [exn])
                        dve(lambda e: e.tensor_tensor(out=ex, in0=ex, in1=eq, op=ALU.mult), [exn, eqn], [exn])
                        dve(lambda e: e.tensor_reduce(out=den[:, 0:1], in_=ex, axis=AX.X, op=ALU.add), [exn], [denn])
                        dve(lambda e: e.reciprocal(out=den[:, 0:1], in_=den[:, 0:1]), [denn], [denn])
                        dve(lambda e: e.tensor_scalar(out=ex, in0=ex, scalar1=den[:, 0:1], scalar2=None, op0=ALU.mult), [exn, denn], [exn])
                        pt_, ptn = nps()
                        k.op("pe", lambda e, pt_=pt_: e.transpose(pt_[0:8, 0:128], ex, identf), reads=[exn, "cst"], writes=[ptn])
                        dve(lambda e, pt_=pt_, c0=c0: e.tensor_copy(out=gT[:, c0:c0 + 128], in_=pt_[0:8, 0:128]), [ptn], [gTn])
            if route:
                dma("sp", gate_in, gT, reads=[gTn], writes=["gate_in"])
                cc("AllGather", gate_in, gate_all, ["gate_in"], ["gate_all"])

        def swiglu_tile(hx, hxn, ld13, ld2, nfc, bufs, ycb):
            (w13t, w13n, w2t, w2n, s1, s1n, g, gn) = bufs
            for fc in range(nfc):
                b = fc % 2
                ld13(fc, w13t[b], w13n[b])
                p1, p1n = nps()
                mm(p1[:, 0:TW], [(w13t[b][:, dc, 0:128], hx[:, dc, :]) for dc in range(DC)], [w13n[b], hxn], [p1n])
                p3, p3n = nps()
                mm(p3[:, 0:TW], [(w13t[b][:, dc, 128:256], hx[:, dc, :]) for dc in range(DC)], [w13n[b], hxn], [p3n])
                act(lambda e, p1=p1, b=b: e.activation(out=s1[b], in_=p1[:, 0:TW], func=AF.Silu), [p1n], [s1n[b]])
                dve(lambda e, p3=p3, b=b, fc=fc: e.tensor_tensor(out=g[:, fc, :], in0=p3[:, 0:TW], in1=s1[b], op=ALU.mult),
                    [p3n, s1n[b]], [gn])
            for dc in range(DC):
                b = dc % 2
                ld2(dc, w2t[b], w2n[b])
                py, pyn = nps()
                mm(py[:, 0:TW], [(w2t[b][:, fc, :], g[:, fc, :]) for fc in range(nfc)], [w2n[b], gn], [pyn])
                ycb(dc, py, pyn)

        def swiglu_bufs(nfc):
            w13t, w13n, w2t, w2n, s1, s1n = [], [], [], [], [], []
            for b in range(2):
                t_, n_ = A.alloc(f"w13t{b}", [128, DC, 256], BF16); w13t.append(t_); w13n.append(n_)
                t_, n_ = A.alloc(f"w2t{b}", [128, nfc, 128], BF16); w2t.append(t_); w2n.append(n_)
                t_, n_ = A.alloc(f"s1{b}", [128, TW]); s1.append(t_); s1n.append(n_)
            g, gn = A.alloc("g", [128, nfc, TW], BF16)
            return (w13t, w13n, w2t, w2n, s1, s1n, g, gn)

        def layer(l):
            norm_mod(l, 0, 1)
            ag_chunked(hown, "hown", hfull, "hfull", RCH)
            phase()
            wpc, wpcn = A.alloc("wpc", [128, DC, 516], BF16)
            H, Hn = A.alloc("H", [128, DC, TW], BF16)
            zf, zfn = A.alloc("zf", [128, TW])
            zg, zgn = A.alloc("zg", [128, TW])
            vt, vtn = A.alloc("vt", [128, 258], BF16)
            sot, sotn = A.alloc("sot", [128, 256], BF16)
            of, ofn = A.alloc("of", [128, 256])
            zpad, zpadn = A.alloc("zpad", [128, 2, 15])
            dve(lambda e: e.memset(vt[:, 256:258], 1.0), [], [vtn])
            dve(lambda e: e.memset(zpad, 0.0), [], [zpadn])
            dma("sp", btc[:], b_tp_col[:, l * 8:(l + 1) * 8], writes=["btc"])
            dma("sp", btr[:], b_tp_row[l].partition_broadcast(128), writes=["btr"])
            dma("sp", fm(ud)[:, :, 0:15], zpad, reads=[zpadn], writes=["ud"])
            dma("sp", fm(ud)[:, :, S + 15:S + 30], zpad, reads=[zpadn], writes=["ud"])
            groups = [(0, 512), (1028, 512), (512, 516)]
            for gi, (gc0, gw) in enumerate(groups):
                for dc in range(DC):
                    dma("pool", wpc[:, dc, 0:gw], w_in_tp[l, dc * 128:(dc + 1) * 128, gc0:gc0 + gw], writes=[wpcn])
                for seg in range(4):
                    for t5 in range(T // TW):
                        g0 = seg * T + t5 * TW
                        hload(H, Hn, seg, slice(t5 * TW, (t5 + 1) * TW))
                        if gi == 0:
                            for ci in range(4):
                                p_, pn = nps()
                                mm(p_[:, 0:TW], [(wpc[:, dc, ci * 128:(ci + 1) * 128], H[:, dc, :]) for dc in range(DC)], [wpcn, Hn], [pn])
                                act(lambda e, p_=p_, ci=ci: e.activation(out=zf, in_=p_[:, 0:TW], func=AF.Identity, bias=btc[:, ci:ci + 1]),
                                    [pn, "btc"], [zfn])
                                dma("sp", zqk[ci * 128:(ci + 1) * 128, g0:g0 + TW], zf, reads=[zfn], writes=["zqk"])
                        elif gi == 1:
                            for hh in range(2):
                                pa, pan = nps()
                                mm(pa[:, 0:TW], [(wpc[:, dc, hh * 128:(hh + 1) * 128], H[:, dc, :]) for dc in range(DC)], [wpcn, Hn], [pan])
                                pb, pbn = nps()
                                mm(pb[:, 0:TW], [(wpc[:, dc, 256 + hh * 128:384 + hh * 128], H[:, dc, :]) for dc in range(DC)], [wpcn, Hn], [pbn])
                                act(lambda e, pb=pb, hh=hh: e.activation(out=zg, in_=pb[:, 0:TW], func=AF.Sigmoid, bias=btc[:, 6 + hh:7 + hh]),
                                    [pbn, "btc"], [zgn])
                                dve(lambda e, pa=pa, hh=hh: e.scalar_tensor_tensor(
                                    out=zf, in0=pa[:, 0:TW], scalar=btc[:, 4 + hh:5 + hh], in1=zg, op0=ALU.add, op1=ALU.mult),
                                    [pan, zgn, "btc"], [zfn])
                                dma("sp", ud[hh * 128:(hh + 1) * 128, 15 + g0:15 + g0 + TW], zf, reads=[zfn], writes=["ud"])
                        else:
                            for tb in range(TW // 128):
                                ch = (g0 + tb * 128) // 128
                                p_, pn = nps()
                                mm(p_[:, :], [(H[:, dc, tb * 128:(tb + 1) * 128], wpc[:, dc, 0:512]) for dc in range(DC)], [wpcn, Hn], [pn])
                                dve(lambda e, p_=p_: e.tensor_tensor(out=vt[:, 0:256], in0=p_[:, 0:256], in1=btr[:, 0:256], op=ALU.add),
                                    [pn, "btr"], [vtn])
                                dve(lambda e, p_=p_: e.tensor_tensor(out=of, in0=p_[:, 256:512], in1=btr[:, 256:512], op=ALU.add),
                                    [pn, "btr"], [ofn])
                                act(lambda e: e.activation(out=sot, in_=of, func=AF.Sigmoid), [ofn], [sotn])
                                dma("sp", vd[ch * 128:(ch + 1) * 128, :], vt, reads=[vtn], writes=["vd"])
                                dma("sp", sod[ch * 128:(ch + 1) * 128, :], sot, reads=[sotn], writes=["sod"])
                                pg, pgn = nps()
                                mm(pg[:, 0:4], [(H[:, dc, tb * 128:(tb + 1) * 128], wpc[:, dc, 512:516]) for dc in range(DC)], [wpcn, Hn], [pgn])
                                dve(lambda e, pg=pg, ch=ch: e.tensor_tensor(out=g4[:, ch, :], in0=pg[:, 0:4], in1=btr[:, 512:516], op=ALU.add),
                                    [pgn, "btr"], ["g4"])
            phase()
            zt, ztn = A.alloc("zt", [128, 4, 514])
            qkt, qktn = A.alloc("qkt", [128, 4, 512], BF16)
            acc, accn = A.alloc("acc", [128, 512])
            dma("sp", wq[:], wqk[:, l * 12:(l + 1) * 12], writes=["wq"])
            dve(lambda e: e.tensor_scalar(out=wq[:, 6:12], in0=wq[:, 6:12], scalar1=1.0 / 16.0, scalar2=None, op0=ALU.mult),
                ["wq"], ["wq"])
            dma("sp", mngb[:], mng[l].partition_broadcast(128), writes=["mngb"])
            n5 = S // 512
            zq3 = fm(zqk)
            for t5 in range(n5):
                g0 = t5 * 512
                if t5 == 0:
                    dve(lambda e: e.memset(zt[:, :, 0:1], 0.0), [], [ztn])
                    dma("sp", zt[:, :, 1:514], zq3[:, :, 0:513], reads=["zqk"], writes=[ztn])
                elif t5 == n5 - 1:
                    dve(lambda e: e.memset(zt[:, :, 513:514], 0.0), [], [ztn])
                    dma("sp", zt[:, :, 0:513], zq3[:, :, g0 - 1:S], reads=["zqk"], writes=[ztn])
                else:
                    dma("sp", zt, zq3[:, :, g0 - 1:g0 + 513], reads=["zqk"], writes=[ztn])
                for ch in range(4):
                    dve(lambda e, ch=ch: e.tensor_scalar(out=acc, in0=zt[:, ch, 0:512], scalar1=wq[:, ch * 3:ch * 3 + 1],
                                                         scalar2=None, op0=ALU.mult), [ztn, "wq"], [accn])
                    dve(lambda e, ch=ch: e.scalar_tensor_tensor(out=acc, in0=zt[:, ch, 1:513], scalar=wq[:, ch * 3 + 1:ch * 3 + 2],
                                                                in1=acc, op0=ALU.mult, op1=ALU.add), [ztn, "wq", accn], [accn])
                    dve(lambda e, ch=ch: e.scalar_tensor_tensor(out=qkt[:, ch, :], in0=zt[:, ch, 2:514], scalar=wq[:, ch * 3 + 2:ch * 3 + 3],
                                                                in1=acc, op0=ALU.mult, op1=ALU.add), [ztn, "wq", accn], [qktn])
                dma("sp", fm(qkd)[:, :, g0:g0 + 512], qkt, reads=[qktn], writes=["qkd"])
            act(lambda e: e.activation(out=tmpg[:, :, 0], in_=g4[:, :, 1], func=AF.Exp, scale=-1.0), ["g4"], ["tmpg"])
            act(lambda e: e.activation(out=tmpg[:, :, 1], in_=g4[:, :, 3], func=AF.Exp, scale=-1.0), ["g4"], ["tmpg"])
            act(lambda e: e.activation(out=lf[:], in_=tmpg[:], func=AF.Ln, bias=1.0), ["tmpg"], ["lf"])
            dve(lambda e: e.tensor_scalar(out=lf[:], in0=lf[:], scalar1=-1.0, scalar2=None, op0=ALU.mult), ["lf"], ["lf"])
            phase()
            qc, qcn = A.alloc("qc", [128, 4, 128], BF16)
            vc, vcn = A.alloc("vc", [128, 258], BF16)
            lfrep, lfrepn = A.alloc("lfrep", [128, 128])
            imb, imbn = A.alloc("imb", [128, 2])
            Bm, Bmn = A.alloc("Bm", [128, 128])
            DT, DTn = A.alloc("DT", [128, 128])
            Wb, Wbn = A.alloc("Wb", [128, 128])
            bl, bln = A.alloc("bl", [128, 2])
            ws, wsn = A.alloc("ws", [128, 2])
            dec, decn = A.alloc("dec", [128, 2])
            PT, PTn = A.alloc("PT", [128, 128], BF16)
            qs, qsn = A.alloc("qs", [128, 2, 128], BF16)
            rd, rdn = A.alloc("rd", [128, 2])
            hc, hcn = A.alloc("hc", [128, 256])
            hfw, hfwn = A.alloc("hfw", [128, 256])
            junk, junkn = A.alloc("junk", [128, 256])
            ssq, ssqn = A.alloc("ssq", [128, 2])
            soc, socn = A.alloc("soc", [128, 256], BF16)
            hmb, hmbn = A.alloc("hmb", [128, 256], BF16)
            hmt, hmtn = A.alloc("hmt", [128, 2, 128], BF16)
            kw, kwn = A.alloc("kw", [128, 256], BF16)
            Cst, Cstn = A.alloc("Cst", [128, 2, 258])
            Cbf, Cbfn = A.alloc("Cbf", [128, 2, 258], BF16)
            for dr in range(2):
                Mmat = Umat if dr == 0 else Lmat
                negm = negf if dr == 0 else negb
                last = 127 if dr == 0 else 0
                dve(lambda e: e.memset(Cst, 0.0), [], [Cstn])
                dve(lambda e: e.memset(Cbf, 0.0), [], [Cbfn])
                order = list(range(NCH)) if dr == 0 else list(reversed(range(NCH)))
                for c in order:
                    rows = slice(c * 128, (c + 1) * 128)
                    icol = g4[:, c, 2 * dr:2 * dr + 1]
                    lfcol = lf[:, c, dr:dr + 1]
                    dma("sp", qc, fm(qkd)[:, :, rows], reads=["qkd"], writes=[qcn])
                    dma("sp", vc, vd[rows, :], reads=["vd"], writes=[vcn])
                    dve(lambda e, lfcol=lfcol: e.tensor_scalar(out=lfrep, in0=onesf, scalar1=lfcol, scalar2=None, op0=ALU.mult),
                        ["cst", "lf"], [lfrepn])
                    pB, pBn = nps()
                    mm(pB[:, 0:128], [(lfrep, Mmat)], [lfrepn, "cst"], [pBn])
                    pb_, pbn_ = nps()
                    mm(pb_[:, 0:1], [(Mmat, lfcol)], ["cst", "lf"], [pbn_])
                    dve(lambda e, icol=icol, pb_=pb_: e.tensor_tensor(out=imb[:, 0:1], in0=icol, in1=pb_[:, 0:1], op=ALU.subtract),
                        ["g4", pbn_], [imbn])
                    dve(lambda e, pB=pB, negm=negm: e.tensor_tensor(out=Bm, in0=pB[:, 0:128], in1=negm, op=ALU.add),
                        [pBn, "cst"], [Bmn])
                    act(lambda e: e.activation(out=DT, in_=Bm, func=AF.Exp, bias=imb[:, 0:1]), [Bmn, imbn], [DTn])
                    act(lambda e, pB=pB: e.activation(out=Wb, in_=pB[:, 0:128], func=AF.Exp), [pBn], [Wbn])
                    dve(lambda e, pB=pB, last=last: e.tensor_copy(out=bl[:, 0:1], in_=pB[:, last:last + 1]), [pBn], [bln])
                    act(lambda e: e.activation(out=ws[:, 0:1], in_=imb[:, 0:1], func=AF.Exp, bias=bl[:, 0:1]), [imbn, bln], [wsn])
                    act(lambda e: e.activation(out=dec[:, 0:1], in_=bl[:, 0:1], func=AF.Exp), [bln], [decn])
                    pS, pSn = nps()
                    mm(pS[:, 0:128], [(qc[:, 2, :], qc[:, 0, :]), (qc[:, 3, :], qc[:, 1, :])], [qcn], [pSn])
                    dve(lambda e, pS=pS: e.tensor_tensor(out=PT, in0=pS[:, 0:128], in1=DT, op=ALU.mult), [pSn, DTn], [PTn])
                    for h in range(2):
                        dve(lambda e, h=h: e.tensor_tensor(out=qs[:, h, :], in0=qc[:, h, :], in1=Wb, op=ALU.mult), [qcn, Wbn], [qsn])
                    pN, pNn = nps()
                    mm(pN[:, 0:257], [(PT, vc[:, 0:257]), (qs[:, 0, :], Cbf[:, 0, 0:257]), (qs[:, 1, :], Cbf[:, 1, 0:257])],
                       [PTn, vcn, qsn, Cbfn], [pNn])
                    act(lambda e, pN=pN: e.activation(out=rd[:, 0:1], in_=pN[:, 256:257], func=AF.Abs), [pNn], [rdn])
                    dve(lambda e: e.tensor_scalar(out=rd[:, 0:1], in0=rd[:, 0:1], scalar1=1.0, scalar2=None, op0=ALU.max), [rdn], [rdn])
                    dve(lambda e: e.reciprocal(out=rd[:, 0:1], in_=rd[:, 0:1]), [rdn], [rdn])
                    if dr == 0:
                        dve(lambda e, pN=pN: e.tensor_scalar(out=hc, in0=pN[:, 0:256], scalar1=rd[:, 0:1], scalar2=None, op0=ALU.mult),
                            [pNn, rdn], [hcn])
                        dma("sp", hfd[rows, :], hc, reads=[hcn], writes=["hfd"])
                    else:
                        dma("sp", hfw, hfd[rows, :], reads=["hfd"], writes=[hfwn])
                        dma("sp", soc, sod[rows, :], reads=["sod"], writes=[socn])
                        dve(lambda e, pN=pN: e.scalar_tensor_tensor(out=hc, in0=pN[:, 0:256], scalar=rd[:, 0:1], in1=hfw,
                                                                    op0=ALU.mult, op1=ALU.add), [pNn, rdn, hfwn], [hcn])
                        dve(lambda e: e.tensor_tensor(out=junk, in0=hc, in1=hc, op=ALU.mult), [hcn], [junkn])
                        dve(lambda e: e.tensor_reduce(out=ssq[:, 0:1], in_=junk, axis=AX.X, op=ALU.add), [junkn], [ssqn])
                        act(lambda e: e.activation(out=ssq[:, 0:1], in_=ssq[:, 0:1], func=AF.Sqrt, scale=1.0 / 256.0, bias=EPS),
                            [ssqn], [ssqn])
                        dve(lambda e: e.reciprocal(out=ssq[:, 0:1], in_=ssq[:, 0:1]), [ssqn], [ssqn])
                        dve(lambda e: e.scalar_tensor_tensor(out=hc, in0=hc, scalar=ssq[:, 0:1], in1=mngb[:],
                                                             op0=ALU.mult, op1=ALU.mult), [hcn, ssqn, "mngb"], [hcn])
                        dve(lambda e: e.tensor_tensor(out=hmb, in0=hc, in1=soc, op=ALU.mult), [hcn, socn], [hmbn])

                        def tr2(e):
                            e.transpose(pst[:, 256:384], hmb[:, 0:128], identb[:])
                            return e.transpose(pst[:, 384:512], hmb[:, 128:256], identb[:])
                        k.op("pe", tr2, reads=[hmbn, "identb"], writes=["pstB"])
                        for h in range(2):
                            dve(lambda e, h=h: e.tensor_copy(out=hmt[:, h, :], in_=pst[:, 256 + h * 128:384 + h * 128]), ["pstB"], [hmtn])
                        dma("sp", fm(hmd)[:, :, rows], hmt, reads=[hmtn], writes=["hmd"])

                    def tr1(e):
                        e.transpose(pst[:, 0:128], qc[:, 2, :], identb[:])
                        return e.transpose(pst[:, 128:256], qc[:, 3, :], identb[:])
                    k.op("pe", tr1, reads=[qcn, "identb"], writes=["pstA"])
                    dve(lambda e: e.tensor_scalar(out=kw, in0=pst[:, 0:256], scalar1=ws[:, 0:1], scalar2=None, op0=ALU.mult),
                        ["pstA", wsn], [kwn])
                    for h in range(2):
                        pC, pCn = nps()
                        mm(pC[:, 0:257], [(kw[:, h * 128:(h + 1) * 128], vc[:, 0:257])], [kwn, vcn], [pCn])
                        dve(lambda e, h=h, pC=pC: e.scalar_tensor_tensor(out=Cst[:, h, 0:257], in0=Cst[:, h, 0:257], scalar=dec[:, 0:1],
                                                                         in1=pC[:, 0:257], op0=ALU.mult, op1=ALU.add), [Cstn, decn, pCn], [Cstn])
                    dve(lambda e: e.tensor_copy(out=Cbf, in_=Cst), [Cstn], [Cbfn])
            phase()
            ut, utn = A.alloc("ut", [128, 2, 542])
            cacc, caccn = A.alloc("cacc", [128, 2, 512])
            sqc, sqcn = A.alloc("sqc", [128, 2, 512])
            uct, uctn = A.alloc("uct", [128, 2, 512], BF16)
            strow, strown = A.alloc("strow", [1, 2, 512])
            st4, st4n = A.alloc("st4", [4, 2, 512])
            mean, meann = A.alloc("mean", [128, 512])
            rsd, rsdn = A.alloc("rsd", [128, 512])
            dma("sp", wdw[:], w_dw[:, l * 62:(l + 1) * 62], writes=["wdw"])
            dma("sp", cv[:], cvec[:, l * 6:(l + 1) * 6], writes=["cv"])
            for t5 in range(S // 512):
                g0 = t5 * 512
                dma("sp", ut, fm(ud)[:, :, g0:g0 + 542], reads=["ud"], writes=[utn])
                for hh in range(2):
                    dve(lambda e, hh=hh: e.tensor_scalar(out=cacc[:, hh, :], in0=ut[:, hh, 0:512], scalar1=wdw[:, hh * 31:hh * 31 + 1],
                                                         scalar2=cv[:, hh * 3:hh * 3 + 1], op0=ALU.mult, op1=ALU.add),
                        [utn, "wdw", "cv"], [caccn])
                    for j in range(1, 31):
                        dve(lambda e, hh=hh, j=j: e.scalar_tensor_tensor(out=cacc[:, hh, :], in0=ut[:, hh, j:j + 512],
                                                                         scalar=wdw[:, hh * 31 + j:hh * 31 + j + 1], in1=cacc[:, hh, :],
                                                                         op0=ALU.mult, op1=ALU.add), [utn, "wdw", caccn], [caccn])
                dma("sp", fm(cvd)[:, :, g0:g0 + 512], cacc, reads=[caccn], writes=["cvd"])
                act(lambda e: e.activation(out=sqc, in_=cacc, func=AF.Square), [caccn], [sqcn])
                p1, p1n = nps()
                mm(p1[:, :], [(onesf, cacc[:, 0, :]), (onesf, cacc[:, 1, :])], ["cst", caccn], [p1n])
                p2, p2n = nps()
                mm(p2[:, :], [(onesf, sqc[:, 0, :]), (onesf, sqc[:, 1, :])], ["cst", sqcn], [p2n])
                dve(lambda e, p1=p1: e.tensor_copy(out=strow[:, 0, :], in_=p1[0:1, :]), [p1n], [strown])
                dve(lambda e, p2=p2: e.tensor_copy(out=strow[:, 1, :], in_=p2[0:1, :]), [p2n], [strown])
                dma("sp", st1_in[:, g0:g0 + 512], strow[:, 0, :], reads=[strown], writes=["st1_in"])
                dma("sp", st2_in[:, g0:g0 + 512], strow[:, 1, :], reads=[strown], writes=["st2_in"])
            cc("AllGather", st1_in, st1_all, ["st1_in"], ["st1_all"])
            cc("AllGather", st2_in, st2_all, ["st2_in"], ["st2_all"])
            for t5 in range(S // 512):
                g0 = t5 * 512
                dma("sp", st4[:, 0, :], st1_all[:, g0:g0 + 512], reads=["st1_all"], writes=[st4n])
                dma("sp", st4[:, 1, :], st2_all[:, g0:g0 + 512], reads=["st2_all"], writes=[st4n])
                dma("sp", cacc, fm(cvd)[:, :, g0:g0 + 512], reads=["cvd"], writes=[caccn])
                p1, p1n = nps()
                mm(p1[:, :], [(onesf[0:4, :], st4[:, 0, :])], ["cst", st4n], [p1n])
                p2, p2n = nps()
                mm(p2[:, :], [(onesf[0:4, :], st4[:, 1, :])], ["cst", st4n], [p2n])
                dve(lambda e, p1=p1: e.tensor_scalar(out=mean, in0=p1[:, :], scalar1=1.0 / 1024.0, scalar2=None, op0=ALU.mult),
                    [p1n], [meann])
                dve(lambda e: e.tensor_tensor(out=rsd, in0=mean, in1=mean, op=ALU.mult), [meann], [rsdn])
                dve(lambda e, p2=p2: e.scalar_tensor_tensor(out=rsd, in0=p2[:, :], scalar=1.0 / 1024.0, in1=rsd,
                                                            op0=ALU.mult, op1=ALU.subtract), [p2n, rsdn], [rsdn])
                act(lambda e: e.activation(out=rsd, in_=rsd, func=AF.Sqrt, bias=EPS), [rsdn], [rsdn])
                dve(lambda e: e.reciprocal(out=rsd, in_=rsd), [rsdn], [rsdn])
                for hh in range(2):
                    dve(lambda e, hh=hh: e.tensor_tensor(out=cacc[:, hh, :], in0=cacc[:, hh, :], in1=mean, op=ALU.subtract),
                        [caccn, meann], [caccn])
                    dve(lambda e, hh=hh: e.tensor_tensor(out=cacc[:, hh, :], in0=cacc[:, hh, :], in1=rsd, op=ALU.mult),
                        [caccn, rsdn], [caccn])
                    dve(lambda e, hh=hh: e.tensor_scalar(out=cacc[:, hh, :], in0=cacc[:, hh, :], scalar1=cv[:, hh * 3 + 1:hh * 3 + 2],
                                                         scalar2=cv[:, hh * 3 + 2:hh * 3 + 3], op0=ALU.mult, op1=ALU.add),
                        [caccn, "cv"], [caccn])
                    act(lambda e, hh=hh: e.activation(out=uct[:, hh, :], in_=cacc[:, hh, :], func=AF.Silu), [caccn], [uctn])
                dma("sp", fm(ucd)[:, :, g0:g0 + 512], uct, reads=[uctn], writes=["ucd"])
            phase()
            wpj, wpjn = A.alloc("wpj", [128, 2, 2, D], BF16)
            act_t, act_tn = A.alloc("actt", [128, 2, 2, TW], BF16)
            ysb, ysbn = A.alloc("ysb", [128, DC, TW])
            for hh in range(2):
                cast_load(wpj[:, 0, hh, :].rearrange("p (a n) -> p a n", n=128), w_mp[l, hh * 128:(hh + 1) * 128, :], 128, [], [wpjn])
                cast_load(wpj[:, 1, hh, :].rearrange("p (a n) -> p a n", n=128), w_cp[l, hh * 128:(hh + 1) * 128, :], 128, [], [wpjn])
            for seg in range(4):
                for t5 in range(T // TW):
                    g0 = seg * T + t5 * TW
                    dma("sp", act_t[:, 0, :, :], fm(hmd)[:, :, g0:g0 + TW], reads=["hmd"], writes=[act_tn])
                    dma("sp", act_t[:, 1, :, :], fm(ucd)[:, :, g0:g0 + TW], reads=["ucd"], writes=[act_tn])
                    for mc in range(2):
                        for dc in range(DC):
                            p_, pn = nps()
                            mm(p_[:, 0:TW], [(wpj[:, mc, hh, dc * 128:(dc + 1) * 128], act_t[:, mc, hh, :]) for hh in range(2)],
                               [wpjn, act_tn], [pn])
                            act(lambda e, p_=p_, dc=dc: e.activation(out=ysb[:, dc, :], in_=p_[:, 0:TW], func=AF.Identity), [pn], [ysbn])
                        r0 = (seg * 2 + mc) * D
                        dma("sp", fm(rs_in[r0:r0 + D, :])[:, :, t5 * TW:(t5 + 1) * TW], ysb, reads=[ysbn], writes=["rs_in"])
            cc("ReduceScatter", rs_in, rs_out, ["rs_in"], ["rs_out"])
            phase()
            hx7, hx7n = A.alloc("hx7", [128, DC, TW], BF16)
            mg, mgn = A.alloc("mg", [128, DC, TW], BF16)
            xt7, xt7n = A.alloc("xt7", [128, DC, TW])
            wg = [A.alloc(f"wg{b}", [128, DC, 128], BF16) for b in range(4)]
            ymc, ymcn = A.alloc("ymc", [128, 2, TW])
            gmc, gmcn = A.alloc("gmc", [128, 2, TW])
            for tt in range(T // TW):
                tsl = slice(tt * TW, (tt + 1) * TW)
                dma("sp", hx7, fm(hown)[:, :, tsl], reads=["hown"], writes=[hx7n])
                dma("sp", xt7, fm(xres)[:, :, tsl], reads=["xres"], writes=[xt7n])
                for dc in range(DC):
                    for mc in range(2):
                        cb = mc * DC + dc
                        wt_, wtn_ = wg[(2 * dc + mc) % 4]
                        cast_load(wt_, gtile(wbr[l], R_BR, cb), 128, [f"wbr{l}"], [wtn_])
                        p_, pn = nps()
                        mm(p_[:, 0:TW], [(wt_[:, kc, :], hx7[:, kc, :]) for kc in range(DC)], [wtn_, hx7n], [pn])
                        bcol = bbr[:, l * 2 * DC + cb:l * 2 * DC + cb + 1]
                        act(lambda e, p_=p_, mc=mc, bcol=bcol: e.activation(out=gmc[:, mc, :], in_=p_[:, 0:TW], func=AF.Sigmoid, bias=bcol),
                            [pn, "bbr"], [gmcn])
                        dma("sp", ymc[:, mc, :], rs_out[mc * D + dc * 128:mc * D + (dc + 1) * 128, tsl], reads=["rs_out"], writes=[ymcn])
                    dve(lambda e: e.tensor_tensor(out=gmc, in0=gmc, in1=ymc, op=ALU.mult), [gmcn, ymcn], [gmcn])
                    dve(lambda e, dc=dc: e.tensor_tensor(out=mg[:, dc, :], in0=gmc[:, 0, :], in1=gmc[:, 1, :], op=ALU.add), [gmcn], [mgn])
                for dc in range(DC):
                    wt_, wtn_ = wg[dc % 4]
                    cast_load(wt_, gtile(wo[l], R_WO, dc), 128, [f"wo{l}"], [wtn_])
                    p_, pn = nps()
                    mm(p_[:, 0:TW], [(wt_[:, kc, :], mg[:, kc, :]) for kc in range(DC)], [wtn_, mgn], [pn])
                    dve(lambda e, p_=p_, dc=dc: e.scalar_tensor_tensor(out=xt7[:, dc, :], in0=p_[:, 0:TW], scalar=mcol(modall, l, 2, dc),
                                                                       in1=xt7[:, dc, :], op0=ALU.mult, op1=ALU.add), [pn, "modall", xt7n], [xt7n])
                dma("sp", fm(xres)[:, :, tsl], xt7, reads=[xt7n], writes=["xres"])
            if stop_after == f"mix{l}":
                return True
            if l % 2 == 0:
                norm_mod(l, 3, 4)
                phase()
                bufs = swiglu_bufs(FC)
                hx8, hx8n = A.alloc("hx8", [128, DC, TW], BF16)
                xt8, xt8n = A.alloc("xt8", [128, DC, TW])
                for tt in range(T // TW):
                    tsl = slice(tt * TW, (tt + 1) * TW)
                    dma("sp", hx8, fm(hown)[:, :, tsl], reads=["hown"], writes=[hx8n])
                    dma("sp", xt8, fm(xres)[:, :, tsl], reads=["xres"], writes=[xt8n])

                    def ycb(dc, py, pyn):
                        dve(lambda e, py=py, dc=dc: e.scalar_tensor_tensor(out=xt8[:, dc, :], in0=py[:, 0:TW], scalar=mcol(modall, l, 5, dc),
                                                                           in1=xt8[:, dc, :], op0=ALU.mult, op1=ALU.add), [pyn, "modall", xt8n], [xt8n])
                    def ld13(fc, t_, tn_):
                        cast_load(t_[:, :, 0:128], gtile(f1, R_F1, fc), 128, ["f1"], [tn_])
                        cast_load(t_[:, :, 128:256], gtile(f3, R_F1, fc), 128, ["f3"], [tn_])

                    def ld2(dc, t_, tn_):
                        for g_ in range(NG):
                            cast_load(t_[:, g_ * GF:(g_ + 1) * GF, :], gtile(f2, R_F2, dc * NG + g_), 128, ["f2"], [tn_])
                    swiglu_tile(hx8, hx8n, ld13, ld2, FC, bufs, ycb)
                    dma("sp", fm(xres)[:, :, tsl], xt8, reads=[xt8n], writes=["xres"])
            else:
                norm_mod(l, 3, 4, route=True)
                ag_chunked(hown, "hown", hfull, "hfull", RCH)
                phase()
                bufs = swiglu_bufs(FCE)
                hx9, hx9n = A.alloc("hx9", [128, DC, TW], BF16)
                yacc, yaccn = A.alloc("yacc", [128, DC, TW])
                gts, gtsn = A.alloc("gts", [8, TW])
                gbt, gbtn = A.alloc("gbt", [128, TW])
                tmpy, tmpyn = A.alloc("tmpy", [128, TW])
                for seg in range(4):
                    for tt in range(T // TW):
                        tsl = slice(tt * TW, (tt + 1) * TW)
                        hload(hx9, hx9n, seg, tsl)
                        dma("sp", gts, gate_all[seg * 8:(seg + 1) * 8, tsl], reads=["gate_all"], writes=[gtsn])
                        for j in range(2):
                            pgb, pgbn = nps()
                            mm(pgb[:, 0:TW], [(selt[:, j * 128:(j + 1) * 128], gts)], ["selt", gtsn], [pgbn])
                            dve(lambda e, pgb=pgb: e.tensor_copy(out=gbt, in_=pgb[:, 0:TW]), [pgbn], [gbtn])

                            def ycb(dc, py, pyn, j=j):
                                if j == 0:
                                    dve(lambda e, py=py, dc=dc: e.tensor_tensor(out=yacc[:, dc, :], in0=py[:, 0:TW], in1=gbt, op=ALU.mult),
                                        [pyn, gbtn], [yaccn])
                                else:
                                    dve(lambda e, py=py: e.tensor_tensor(out=tmpy, in0=py[:, 0:TW], in1=gbt, op=ALU.mult), [pyn, gbtn], [tmpyn])
                                    dve(lambda e, dc=dc: e.tensor_tensor(out=yacc[:, dc, :], in0=yacc[:, dc, :], in1=tmpy, op=ALU.add),
                                        [yaccn, tmpyn], [yaccn])
                            def ld13(fc, t_, tn_, j=j):
                                cast_load(t_, moe13[j, fc * 128:(fc + 1) * 128, :], 256, [], [tn_])

                            def ld2(dc, t_, tn_, j=j):
                                cast_load(t_, moe2[j, dc * 128:(dc + 1) * 128, :], 128, [], [tn_])
                            swiglu_tile(hx9, hx9n, ld13, ld2, FCE, bufs, ycb)
                        dma("sp", fm(rs2_in[seg * D:(seg + 1) * D, :])[:, :, tsl], yacc, reads=[yaccn], writes=["rs2_in"])
                cc("ReduceScatter", rs2_in, rs2_out, ["rs2_in"], ["rs2_out"])
                phase()
                xt9, xt9n = A.alloc("xt9", [128, DC, TW])
                mo, mon = A.alloc("mo", [128, DC, TW])
                for tt in range(T // TW):
                    tsl = slice(tt * TW, (tt + 1) * TW)
                    dma("sp", xt9, fm(xres)[:, :, tsl], reads=["xres"], writes=[xt9n])
                    dma("sp", mo, fm(rs2_out)[:, :, tsl], reads=["rs2_out"], writes=[mon])
                    for dc in range(DC):
                        dve(lambda e, dc=dc: e.scalar_tensor_tensor(out=xt9[:, dc, :], in0=mo[:, dc, :], scalar=mcol(modall, l, 5, dc),
                                                                    in1=xt9[:, dc, :], op0=ALU.mult, op1=ALU.add), [mon, "modall", xt9n], [xt9n])
                    dma("sp", fm(xres)[:, :, tsl], xt9, reads=[xt9n], writes=["xres"])
            if stop_after == f"L{l}":
                return True

            return False

        for l_ in range(L):
            if layer(l_):
                break

        def final_out():
            phase()
            xt, xtn = A.alloc("xtf", [128, DC, TTN])
            sqt, sqtn = A.alloc("sqtf", [128, DC, TTN])
            rstd, rstdn = A.alloc("rstdf", [128, TTN])
            for tt in range(T // TTN):
                tsl = slice(tt * TTN, (tt + 1) * TTN)
                dma("sp", xt, fm(xres)[:, :, tsl], reads=["xres"], writes=[xtn])
                if stop_after == "all":
                    act(lambda e: e.activation(out=sqt, in_=xt, func=AF.Square), [xtn], [sqtn])
                    p_, pn = nps()
                    mm(p_[:, 0:TTN], [(onesf, sqt[:, dc, :]) for dc in range(DC)], [sqtn, "cst"], [pn])
                    act(lambda e, p_=p_: e.activation(out=rstd, in_=p_[:, 0:TTN], func=AF.Sqrt, scale=1.0 / D, bias=EPS), [pn], [rstdn])
                    dve(lambda e: e.reciprocal(out=rstd, in_=rstd), [rstdn], [rstdn])
                    for dc in range(DC):
                        dve(lambda e, dc=dc: e.tensor_tensor(out=xt[:, dc, :], in0=xt[:, dc, :], in1=rstd, op=ALU.mult), [xtn, rstdn], [xtn])
                        dve(lambda e, dc=dc: e.tensor_scalar(out=xt[:, dc, :], in0=xt[:, dc, :], scalar1=fgc[:, dc:dc + 1], scalar2=None,
                                                             op0=ALU.mult), [xtn, "fgc"], [xtn])
                dma("sp", fm(yT)[:, :, tsl], xt, reads=[xtn], writes=["yT"])

        final_out()
        k.emit()
    return nc


def make_consts():
    r = np.arange(128)[:, None]
    t = np.arange(128)[None, :]
    U = (r <= t).astype(np.float32)
    Lm = (r >= t).astype(np.float32)
    negf = np.where(t >= r, 0.0, NEG).astype(np.float32)
    negb = np.where(t <= r, 0.0, NEG).astype(np.float32)
    ident = np.eye(128, dtype=np.float32)
    ones = np.ones((128, 128), np.float32)
    return np.concatenate([U, Lm, negf, negb, ident, ones], axis=1)


def col128(v):
    return np.ascontiguousarray(v.reshape(-1, 128).T)


def tile_kn(W, kc, nc_):
    return np.ascontiguousarray(W.reshape(kc, 128, nc_, 128).transpose(2, 1, 0, 3)).reshape(nc_ * 128, kc * 128)


def tile_13(W13, kc, fcn):
    F_ = fcn * 128
    a = W13[:, :F_].reshape(kc, 128, fcn, 128).transpose(2, 1, 0, 3)
    b = W13[:, F_:].reshape(kc, 128, fcn, 128).transpose(2, 1, 0, 3)
    return np.ascontiguousarray(np.concatenate([a, b], axis=3)).reshape(fcn * 128, kc * 256)


def prep_inputs(cfg, inp):
    D, T, L, DFF, DFFE = cfg["D"], cfg["T"], cfg["L"], cfg["DFF"], cfg["DFFE"]
    DC = D // 128
    NMC = 6 * DC // 4
    FC, FCE = DFF // 128, DFFE // 128
    WM, WC = 1024, 1024
    OFF_G = 4 * WM
    OFF_GLU = OFF_G + 16
    OFF_BR = OFF_GLU + 2 * WC
    consts = make_consts()
    wbr_t = [tile_kn(inp["w_in"][l][:, OFF_BR:], DC, 2 * DC) for l in range(L)]
    wo_t = [tile_kn(inp["w_out"][l], DC, DC) for l in range(L)]
    f1_t = tile_kn(inp["ffn_w13"][0][:, :DFF], DC, FC)
    f3_t = tile_kn(inp["ffn_w13"][0][:, DFF:], DC, FC)
    NG = next(n for n in range((FC + 15) // 16, FC + 1) if FC % n == 0)
    GF = FC // NG
    f2_t = np.ascontiguousarray(inp["ffn_w2"][0].reshape(NG, GF, 128, DC, 128).transpose(3, 0, 2, 1, 4)).reshape(DC * NG * 128, GF * 128)
    router_t = np.ascontiguousarray(inp["moe_router"][0].reshape(DC, 128, 8).transpose(1, 0, 2)).reshape(128, DC * 8)
    maps = []
    for r in range(8):
        b, i = r // 4, r % 4
        m = {}
        m["xT"] = np.ascontiguousarray(inp["x"][b, i * T:(i + 1) * T, :].T)
        m["c_col"] = col128(inp["c"][b])
        m["consts"] = consts
        cs = slice(i * NMC * 128, (i + 1) * NMC * 128)
        m["w_mod_q"] = np.ascontiguousarray(inp["w_mod"][:, :, cs])
        m["b_mod_q"] = np.concatenate([col128(inp["b_mod"][l, cs]) for l in range(L)], axis=1)
        hs = slice(i * 256, (i + 1) * 256)
        gcols = [OFF_G + (d * 2 + g) * 4 + i for d in range(2) for g in range(2)]
        cols = np.concatenate([np.arange(0, WM)[hs], np.arange(WM, 2 * WM)[hs], np.arange(2 * WM, 3 * WM)[hs],
                               np.arange(3 * WM, 4 * WM)[hs], np.array(gcols),
                               np.arange(OFF_GLU, OFF_GLU + WC)[hs], np.arange(OFF_GLU + WC, OFF_GLU + 2 * WC)[hs]])
        m["w_in_tp"] = np.ascontiguousarray(inp["w_in"][:, :, cols])
        btp = inp["b_in"][:, cols]
        fc_ = np.concatenate([np.arange(0, 512), np.arange(1028, 1540)])
        m["b_tp_col"] = np.concatenate([col128(btp[l, fc_]) for l in range(L)], axis=1)
        m["b_tp_row"] = np.ascontiguousarray(btp[:, 512:1028])
        qkc = np.concatenate([np.arange(0, WM)[hs], np.arange(WM, 2 * WM)[hs]])
        wq_ = inp["w_qk_conv"][:, :, qkc]
        m["wqk"] = np.concatenate([np.stack([col128(wq_[l, j]) for j in range(3)], axis=2).reshape(128, 12)
                                   for l in range(L)], axis=1)
        m["mng"] = np.ascontiguousarray(inp["m_norm_g"][:, hs])
        m["w_mp"] = np.ascontiguousarray(inp["w_m_proj"][:, hs, :])
        wd = inp["w_dw"][:, :, hs]
        m["w_dw"] = np.concatenate([np.stack([col128(wd[l, j]) for j in range(31)], axis=2).reshape(128, 62)
                                    for l in range(L)], axis=1)
        m["cvec"] = np.concatenate([np.stack([col128(inp[nm][l, hs]) for nm in ("b_dw", "ln_c_g", "ln_c_b")], axis=2).reshape(128, 6)
                                    for l in range(L)], axis=1)
        m["w_cp"] = np.ascontiguousarray(inp["w_c_proj"][:, hs, :])
        m["w_br_q"] = np.stack([np.array_split(wbr_t[l], 4, axis=0)[i] for l in range(L)])
        m["b_br_col"] = np.concatenate([col128(inp["b_in"][l, OFF_BR:]) for l in range(L)], axis=1)
        m["w_out_q"] = np.stack([np.array_split(wo_t[l], 4, axis=0)[i] for l in range(L)])
        m["ffn1_q"] = np.array_split(f1_t, 4, axis=0)[i]
        m["ffn3_q"] = np.array_split(f3_t, 4, axis=0)[i]
        m["ffn2_q"] = np.array_split(f2_t, 4, axis=0)[i]
        m["router"] = router_t
        se = np.zeros((8, 256), np.float32)
        se[2 * i, 0:128] = 1.0
        se[2 * i + 1, 128:256] = 1.0
        m["sele"] = se
        m["moe13"] = np.stack([tile_13(inp["moe_w13"][0, 2 * i + j], DC, FCE) for j in range(2)])
        m["moe2"] = np.stack([tile_kn(inp["moe_w2"][0, 2 * i + j], FCE, DC) for j in range(2)])
        m["fg_col"] = col128(inp["final_g"])
        maps.append({k_: np.ascontiguousarray(v, dtype=np.float32) for k_, v in m.items()})
    return maps


def run(cfg, inp, stop_after="all"):
    nc = build(cfg, stop_after)
    maps = prep_inputs(cfg, inp)
    res = run_bass_kernel_spmd(nc, maps, core_ids=list(range(8)))
    T, D = cfg["T"], cfg["D"]
    out = np.zeros((2, 4 * T, D), np.float32)
    for r in range(8):
        b, i = r // 4, r % 4
        out[b, i * T:(i + 1) * T, :] = res.results[r]["yT"].T
    return out, res


def kernel(**inputs):
    inp = {k_: np.asarray(v) for k_, v in inputs.items()}
    out, _ = run(FULL, inp)
    return out
```
